# Optimizing a Trainium2 kernel written in Bass

```python
import jax, jax.numpy as jnp
from jax import lax
import numpy as np

D_MODEL = 1024
BATCH = 4
SEQ = 8192
DEPTH = 4

MLA_HEADS = 4
MLA_Q_LORA = 384
MLA_KV_LORA = 256
MLA_NOPE_DIM = 128
MLA_ROPE_DIM = 64
MLA_V_DIM = 128
MLA_WIDTH = MLA_HEADS * MLA_V_DIM
ROPE_THETA = 10000.0
Q_BLOCK = 128
MLSTM_HEADS = 4
MLSTM_HEAD_DIM = 64
MLSTM_WIDTH = MLSTM_HEADS * MLSTM_HEAD_DIM
MLSTM_CONV = 5
MLSTM_CHUNK = 64
FNET_GROUPS = 4
FNET_GROUP_DIM = 64
FNET_WIDTH = FNET_GROUPS * FNET_GROUP_DIM
MIX_WIDTH = MLA_WIDTH + MLSTM_WIDTH + FNET_WIDTH
IN_SPLITS = (MLA_Q_LORA, MLA_KV_LORA, MLA_ROPE_DIM, MLSTM_WIDTH, MLSTM_WIDTH, MLSTM_WIDTH, 4 * MLSTM_HEADS, FNET_WIDTH)
D_IN = MLA_Q_LORA + MLA_KV_LORA + MLA_ROPE_DIM + 3 * MLSTM_WIDTH + 4 * MLSTM_HEADS + FNET_WIDTH
D_FF = 2816
PLE_DIM = 256
EPS = 1e-6

kernel_name = 'hybrid_mla_mlstm_fnet_macaron_encoder'


def rmsnorm(x, g):
    xf = x.astype(jnp.float32)
    y = xf * lax.rsqrt(jnp.mean(xf * xf, axis=-1, keepdims=True) + EPS)
    return (y * g.astype(jnp.float32)).astype(x.dtype)


def swiglu(x, w_gate, w_up, w_down):
    return (jax.nn.silu(x @ w_gate) * (x @ w_up)) @ w_down


def split_cols(z, sizes):
    offs = [int(o) for o in np.cumsum(sizes)[:-1]]
    return jnp.split(z, offs, axis=-1)


def rope_tables(positions):
    inv = 1.0 / (ROPE_THETA ** (jnp.arange(0, MLA_ROPE_DIM, 2, dtype=jnp.float32) / MLA_ROPE_DIM))
    ang = positions.astype(jnp.float32)[..., None] * inv
    return jnp.cos(ang), jnp.sin(ang)


def apply_rope(x, cos, sin):
    xf = x.astype(jnp.float32)
    x1, x2 = jnp.split(xf, 2, axis=-1)
    out = jnp.concatenate([x1 * cos - x2 * sin, x2 * cos + x1 * sin], axis=-1)
    return out.astype(x.dtype)


def mla_mixer(c_q, c_kv, k_rope, cos, sin, q_norm, w_uq, kv_norm, w_ukv):
    B, S, _ = c_q.shape
    q = (rmsnorm(c_q, q_norm) @ w_uq).reshape(B, S, MLA_HEADS, MLA_NOPE_DIM + MLA_ROPE_DIM)
    q_nope, q_rope = q[..., :MLA_NOPE_DIM], q[..., MLA_NOPE_DIM:]
    q_rope = apply_rope(q_rope, cos[:, :, None, :], sin[:, :, None, :])
    kv = (rmsnorm(c_kv, kv_norm) @ w_ukv).reshape(B, S, MLA_HEADS, MLA_NOPE_DIM + MLA_V_DIM)
    k_nope, v = kv[..., :MLA_NOPE_DIM], kv[..., MLA_NOPE_DIM:]
    k_rope = apply_rope(k_rope, cos, sin)
    scale = (MLA_NOPE_DIM + MLA_ROPE_DIM) ** -0.5
    nb = S // Q_BLOCK

    def attend(args):
        qn, qr = args
        s = jnp.einsum('bqhd,bkhd->bhqk', qn, k_nope) + jnp.einsum('bqhr,bkr->bhqk', qr, k_rope)
        pr = jax.nn.softmax(s.astype(jnp.float32) * scale, axis=-1).astype(v.dtype)
        return jnp.einsum('bhqk,bkhd->bqhd', pr, v)

    qn_b = jnp.moveaxis(q_nope.reshape(B, nb, Q_BLOCK, MLA_HEADS, MLA_NOPE_DIM), 1, 0)
    qr_b = jnp.moveaxis(q_rope.reshape(B, nb, Q_BLOCK, MLA_HEADS, MLA_ROPE_DIM), 1, 0)
    o = lax.map(attend, (qn_b, qr_b))
    return jnp.moveaxis(o, 0, 1).reshape(B, S, MLA_WIDTH)


def mlstm_chunkwise(q, k, v, i_pre, f_pre):
    B, H, S, d = q.shape
    L = MLSTM_CHUNK
    nc = S // L
    q = q * d ** -0.5
    lf = jax.nn.log_sigmoid(f_pre)
    causal = jnp.tril(jnp.ones((L, L), dtype=bool))

    def chunks(t):
        return jnp.moveaxis(t.reshape((B, H, nc, L) + t.shape[3:]), 2, 0)

    def step(carry, xs):
        C, n, m = carry
        qc, kc, vc, ic, lfc = xs
        b = jnp.cumsum(lfc, axis=-1)
        D = jnp.where(causal, b[..., :, None] - b[..., None, :] + ic[..., None, :], -jnp.inf)
        m_inter = b + m[..., None]
        m_t = jnp.maximum(m_inter, jnp.max(D, axis=-1))
        w_intra = jnp.exp(D - m_t[..., None])
        w_state = jnp.exp(m_inter - m_t)
        s = jnp.einsum('bhtd,bhsd->bhts', qc, kc) * w_intra
        numer = jnp.einsum('bhts,bhsd->bhtd', s, vc) + w_state[..., None] * jnp.einsum('bhvk,bhtk->bhtv', C, qc)
        denom = jnp.sum(s, axis=-1) + w_state * jnp.einsum('bhk,bhtk->bht', n, qc)
        h = numer / jnp.maximum(jnp.abs(denom), jnp.exp(-m_t))[..., None]
        bL = b[..., -1]
        lw = bL[..., None] - b + ic
        m_new = jnp.maximum(bL + m, jnp.max(lw, axis=-1))
        ws = jnp.exp(lw - m_new[..., None])
        dec = jnp.exp(bL + m - m_new)
        C = dec[..., None, None] * C + jnp.einsum('bhs,bhsv,bhsk->bhvk', ws, vc, kc)
        n = dec[..., None] * n + jnp.einsum('bhs,bhsk->bhk', ws, kc)
        return (C, n, m_new), h

    init = (jnp.zeros((B, H, d, d), jnp.float32), jnp.zeros((B, H, d), jnp.float32), jnp.zeros((B, H), jnp.float32))
    _, h = lax.scan(step, init, (chunks(q), chunks(k), chunks(v), chunks(i_pre), chunks(lf)))
    return jnp.moveaxis(h, 0, 2).reshape(B, H, S, d)


def mlstm_mixer(m_x, m_v, m_o, m_if, conv_w, conv_b, w_q, w_k, i_bias, f_bias, head_norm, skip):
    B, S, _ = m_x.shape
    pad = MLSTM_CONV // 2
    xc = lax.conv_general_dilated(m_x, conv_w[:, None, :], window_strides=(1,), padding=[(pad, pad)],
                                  dimension_numbers=('NWC', 'WIO', 'NWC'), feature_group_count=MLSTM_WIDTH)
    xc = jax.nn.silu(xc + conv_b)
    xh = xc.reshape(B, S, MLSTM_HEADS, MLSTM_HEAD_DIM)
    q = jnp.einsum('bshd,hde->bhse', xh, w_q).astype(jnp.float32)
    k = jnp.einsum('bshd,hde->bhse', xh, w_k).astype(jnp.float32)
    v = m_v.reshape(B, S, MLSTM_HEADS, MLSTM_HEAD_DIM).transpose(0, 2, 1, 3).astype(jnp.float32)
    g = m_if.astype(jnp.float32).reshape(B, S, 2, 2, MLSTM_HEADS)
    i_pre = jnp.transpose(g[:, :, 0] + i_bias.astype(jnp.float32), (2, 0, 3, 1))
    f_pre = jnp.transpose(g[:, :, 1] + f_bias.astype(jnp.float32), (2, 0, 3, 1))
    h_fwd = mlstm_chunkwise(q, k, v, i_pre[0], f_pre[0])
    flip = lambda t: jnp.flip(t, axis=2)
    h_bwd = flip(mlstm_chunkwise(flip(q), flip(k), flip(v), flip(i_pre[1]), flip(f_pre[1])))
    h = (h_fwd + h_bwd).transpose(0, 2, 1, 3)
    h = h * lax.rsqrt(jnp.mean(h * h, axis=-1, keepdims=True) + EPS)
    h = h.reshape(B, S, MLSTM_WIDTH) * head_norm.astype(jnp.float32)
    out = (h + skip.astype(jnp.float32) * xc.astype(jnp.float32)) * jax.nn.sigmoid(m_o.astype(jnp.float32))
    return out.astype(m_x.dtype)


def fnet_mixer(z, w, b):
    B, S, _ = z.shape
    zg = z.reshape(B, S, FNET_GROUPS, FNET_GROUP_DIM).astype(jnp.float32)
    y = jnp.fft.fftn(zg, axes=(1, 3), norm='ortho').real.astype(z.dtype)
    return jnp.einsum('bsgc,gce->bsge', y, w).reshape(B, S, FNET_WIDTH) + b


def setup_inputs(seed: int = 0) -> dict:
    key = jax.random.key(seed)
    ks = iter(jax.random.split(key, 40))
    f32 = jnp.float32

    def nrm(shape, fan_in):
        return jax.random.normal(next(ks), shape, f32) * fan_in ** -0.5

    def gain(shape):
        return 1.0 + 0.02 * jax.random.normal(next(ks), shape, f32)

    Ld = DEPTH
    x = jax.random.normal(next(ks), (BATCH, SEQ, D_MODEL), f32)
    p = jax.random.normal(next(ks), (DEPTH, BATCH, SEQ, PLE_DIM), f32)
    offsets = jax.random.randint(next(ks), (BATCH, 1), 0, 4096, dtype=jnp.int32)
    positions = offsets + jnp.arange(SEQ, dtype=jnp.int32)[None, :]
    f_lin = jnp.linspace(3.0, 6.0, MLSTM_HEADS, dtype=f32)
    return {
        'x': x,
        'p': p,
        'positions': positions,
        'ffn1_norm': gain((Ld, D_MODEL)),
        'ffn1_w_gate': nrm((Ld, D_MODEL, D_FF), D_MODEL),
        'ffn1_w_up': nrm((Ld, D_MODEL, D_FF), D_MODEL),
        'ffn1_w_down': nrm((Ld, D_FF, D_MODEL), D_FF),
        'mix_norm': gain((Ld, D_MODEL)),
        'w_in': nrm((Ld, D_MODEL, D_IN), D_MODEL),
        'mla_q_norm': gain((Ld, MLA_Q_LORA)),
        'mla_w_uq': nrm((Ld, MLA_Q_LORA, MLA_HEADS * (MLA_NOPE_DIM + MLA_ROPE_DIM)), MLA_Q_LORA),
        'mla_kv_norm': gain((Ld, MLA_KV_LORA)),
        'mla_w_ukv': nrm((Ld, MLA_KV_LORA, MLA_HEADS * (MLA_NOPE_DIM + MLA_V_DIM)), MLA_KV_LORA),
        'mlstm_conv_w': nrm((Ld, MLSTM_CONV, MLSTM_WIDTH), MLSTM_CONV),
        'mlstm_conv_b': 0.02 * jax.random.normal(next(ks), (Ld, MLSTM_WIDTH), f32),
        'mlstm_w_q': nrm((Ld, MLSTM_HEADS, MLSTM_HEAD_DIM, MLSTM_HEAD_DIM), MLSTM_HEAD_DIM),
        'mlstm_w_k': nrm((Ld, MLSTM_HEADS, MLSTM_HEAD_DIM, MLSTM_HEAD_DIM), MLSTM_HEAD_DIM),
        'mlstm_i_bias': 0.1 * jax.random.normal(next(ks), (Ld, 2, MLSTM_HEADS), f32),
        'mlstm_f_bias': f_lin + 0.1 * jax.random.normal(next(ks), (Ld, 2, MLSTM_HEADS), f32),
        'mlstm_head_norm': gain((Ld, MLSTM_WIDTH)),
        'mlstm_skip': gain((Ld, MLSTM_WIDTH)),
        'fnet_w': nrm((Ld, FNET_GROUPS, FNET_GROUP_DIM, FNET_GROUP_DIM), FNET_GROUP_DIM),
        'fnet_b': 0.02 * jax.random.normal(next(ks), (Ld, FNET_WIDTH), f32),
        'w_out': nrm((Ld, MIX_WIDTH, D_MODEL), MIX_WIDTH),
        'ffn2_norm': gain((Ld, D_MODEL)),
        'ffn2_w_gate': nrm((Ld, D_MODEL, D_FF), D_MODEL),
        'ffn2_w_up': nrm((Ld, D_MODEL, D_FF), D_MODEL),
        'ffn2_w_down': nrm((Ld, D_FF, D_MODEL), D_FF),
        'ple_gate_norm': gain((Ld, D_MODEL)),
        'ple_w_gate': nrm((Ld, D_MODEL, D_MODEL), D_MODEL),
        'ple_w_proj': nrm((Ld, PLE_DIM, D_MODEL), PLE_DIM),
        'ple_post_norm': gain((Ld, D_MODEL)),
        'final_norm': gain((D_MODEL,)),
    }


def reference(x, p, positions, ffn1_norm, ffn1_w_gate, ffn1_w_up, ffn1_w_down, mix_norm, w_in,
              mla_q_norm, mla_w_uq, mla_kv_norm, mla_w_ukv, mlstm_conv_w, mlstm_conv_b, mlstm_w_q, mlstm_w_k,
              mlstm_i_bias, mlstm_f_bias, mlstm_head_norm, mlstm_skip, fnet_w, fnet_b, w_out,
              ffn2_norm, ffn2_w_gate, ffn2_w_up, ffn2_w_down, ple_gate_norm, ple_w_gate, ple_w_proj,
              ple_post_norm, final_norm):
    cos, sin = rope_tables(positions)
    h = x
    for i in range(DEPTH):
        h = h + 0.5 * swiglu(rmsnorm(h, ffn1_norm[i]), ffn1_w_gate[i], ffn1_w_up[i], ffn1_w_down[i])
        u = rmsnorm(h, mix_norm[i]) @ w_in[i]
        c_q, c_kv, k_rope, m_x, m_v, m_o, m_if, f_in = split_cols(u, IN_SPLITS)
        y_mla = mla_mixer(c_q, c_kv, k_rope, cos, sin, mla_q_norm[i], mla_w_uq[i], mla_kv_norm[i], mla_w_ukv[i])
        y_mlstm = mlstm_mixer(m_x, m_v, m_o, m_if, mlstm_conv_w[i], mlstm_conv_b[i], mlstm_w_q[i], mlstm_w_k[i],
                              mlstm_i_bias[i], mlstm_f_bias[i], mlstm_head_norm[i], mlstm_skip[i])
        y_fnet = fnet_mixer(f_in, fnet_w[i], fnet_b[i])
        y = jnp.concatenate([y_mla, y_mlstm, y_fnet], axis=-1)
        h = h + y @ w_out[i]
        h = h + 0.5 * swiglu(rmsnorm(h, ffn2_norm[i]), ffn2_w_gate[i], ffn2_w_up[i], ffn2_w_down[i])
        e = rmsnorm(p[i] @ ple_w_proj[i], ple_post_norm[i])
        gate = jax.nn.sigmoid(rmsnorm(h, ple_gate_norm[i]) @ ple_w_gate[i])
        h = h + gate * e
    return rmsnorm(h, final_norm)
```

```python
import numpy as np
import concourse.bass as bass
import concourse.mybir as mybir
from concourse.bass_utils import run_bass_kernel_spmd

F32 = mybir.dt.float32
BF16 = mybir.dt.bfloat16
I32 = mybir.dt.int32
AF = mybir.ActivationFunctionType
ALU = mybir.AluOpType
AX = mybir.AxisListType

COMPUTE = ("pe", "act", "dve", "pool")


class Buf:
    __slots__ = ("t", "name", "lw", "lr")

    def __init__(self, t, name=""):
        self.t = t
        self.name = name
        self.lw = {}
        self.lr = {}

    def __getitem__(self, idx):
        return self.t[idx]


class Op:
    __slots__ = ("eng", "fn", "deps", "is_dma", "needs_inc", "cnt", "sem", "semval", "prev_semval")

    def __init__(self, eng, fn, is_dma):
        self.eng = eng
        self.fn = fn
        self.deps = set()
        self.is_dma = is_dma
        self.needs_inc = False
        self.cnt = None
        self.sem = None
        self.semval = None
        self.prev_semval = None


class Prog:
    def __init__(self, nc, n_dma_sems=12):
        self.nc = nc
        self.ops = []
        self.n_dma_sems = n_dma_sems
        self._names = 0

    def sb(self, shape, dt=F32, name=None):
        self._names += 1
        t = self.nc.alloc_sbuf_tensor(name or f"sb{self._names}", list(shape), dt)
        return Buf(t, name or f"sb{self._names}")

    def ps(self, shape, dt=F32, name=None):
        self._names += 1
        t = self.nc.alloc_psum_tensor(name or f"ps{self._names}", list(shape), dt)
        return Buf(t, name or f"ps{self._names}")

    def dram(self, name, shape, dt=F32, kind="Internal"):
        t = self.nc.dram_tensor(name, list(shape), dt, kind=kind)
        return Buf(t, name)

    def view(self, t, name=""):
        return Buf(t, name)

    def op(self, eng, fn, reads=(), writes=(), dma=False):
        idx = len(self.ops)
        o = Op(eng, fn, dma)
        key = ("dma", idx) if dma else eng
        deps = {}
        for b in reads:
            for k, i in b.lw.items():
                deps[i] = True
        for b in writes:
            for k, i in b.lw.items():
                deps.setdefault(i, False)
            for k, i in b.lr.items():
                deps.setdefault(i, False)
        for b in reads:
            b.lr[key] = idx
        for b in writes:
            b.lw = {key: idx}
            b.lr = {}
        deps.pop(idx, None)
        keep = set()
        for i, raw in deps.items():
            d = self.ops[i]
            if (not d.is_dma) and (not dma) and d.eng == eng:
                if eng != "pe":
                    keep.add(i)
            else:
                keep.add(i)
        o.deps = keep
        self.ops.append(o)
        return idx

    def mm(self, out_ap, lhsT, rhs, start, stop, reads, writes):
        return self.op("pe", lambda e: e.matmul(out_ap, lhsT, rhs, start=start, stop=stop), reads, writes)

    def dma_(self, q, out_ap, in_ap, reads, writes, **kw):
        return self.op(q, lambda e: e.dma_start(out=out_ap, in_=in_ap, **kw), reads, writes, dma=True)

    def emit(self):
        nc = self.nc
        ops = self.ops
        for j, o in enumerate(ops):
            for i in o.deps:
                if not ops[i].is_dma:
                    ops[i].needs_inc = True
        engs = ["pe", "act", "dve", "pool", "sp"]
        cnt = {e: 0 for e in engs}
        for o in ops:
            if not o.is_dma and o.needs_inc:
                cnt[o.eng] += 1
                o.cnt = cnt[o.eng]
        import contextlib
        with contextlib.ExitStack() as st:
            esem = {e: st.enter_context(nc.semaphore(f"s_{e}")) for e in engs}
            dsem = {e: [st.enter_context(nc.semaphore(f"d_{e}{i}")) for i in range(self.n_dma_sems)]
                    for e in ("sp", "act", "pool")}
            dcount = {e: 0 for e in dsem}
            dval = {e: [0] * self.n_dma_sems for e in dsem}
            for o in ops:
                if o.is_dma:
                    k = dcount[o.eng] % self.n_dma_sems
                    dcount[o.eng] += 1
                    o.sem = dsem[o.eng][k]
                    o.prev_semval = dval[o.eng][k]
                    dval[o.eng][k] += 16
                    o.semval = dval[o.eng][k]
            block = st.enter_context(nc.Block())
            per_eng = {e: [o for o in ops if o.eng == e] for e in engs}

            def run(e, eh):
                waited = {}
                for o in per_eng[e]:
                    need = {}
                    for i in o.deps:
                        d = ops[i]
                        if d.is_dma:
                            s, v = d.sem, d.semval
                        else:
                            s, v = esem[d.eng], d.cnt
                        kk = id(s)
                        if need.get(kk, (None, 0))[1] < v:
                            need[kk] = (s, v)
                    if o.is_dma and o.prev_semval > 0:
                        kk = id(o.sem)
                        if need.get(kk, (None, 0))[1] < o.prev_semval:
                            need[kk] = (o.sem, o.prev_semval)
                    for kk, (s, v) in need.items():
                        if waited.get(kk, 0) < v:
                            eh.wait_ge(s, v)
                            waited[kk] = v
                    ins = o.fn(eh)
                    if o.is_dma:
                        ins.then_inc(o.sem, 16)
                    elif o.needs_inc:
                        ins.then_inc(esem[e], 1)
                if e in dsem:
                    for k in range(self.n_dma_sems):
                        if dval[e][k] > 0 and waited.get(id(dsem[e][k]), 0) < dval[e][k]:
                            eh.wait_ge(dsem[e][k], dval[e][k])

            @block.tensor
            def _(eh):
                run("pe", eh)

            @block.scalar
            def _(eh):
                run("act", eh)

            @block.vector
            def _(eh):
                run("dve", eh)

            @block.gpsimd
            def _(eh):
                run("pool", eh)

            @block.sync
            def _(eh):
                run("sp", eh)
        return nc


EPS = 1e-6
D = 1024
DFF = 2816
NU = 1808
T = 512


class Ctx:
    def __init__(self, P, wbuf_kc=22, prep_n=2816):
        self.P = P
        nc = P.nc
        self.ones_bf = P.sb([128, 128], BF16, "ones_bf")
        P.op("pool", lambda e: e.memset(self.ones_bf[:, :], 1.0), [], [self.ones_bf])
        self.wbuf = [P.sb([128, wbuf_kc, 512], BF16, f"wbuf{i}") for i in range(2)]
        self.wi = 0
        self.lin_ps = [P.ps([128, 512], F32, f"linps{i}") for i in range(6)]
        self.pi = 0
        self.ps_n = P.ps([128, 512], F32, "ps_n")
        self.prep_f = [P.sb([128, prep_n], F32, f"prepf{i}") for i in range(2)]
        self.prep_b = [P.sb([128, prep_n], BF16, f"prepb{i}") for i in range(2)]
        self.prep_i = 0
        self.sq = P.sb([128, 8, T], BF16, "sq")
        self.sd = P.sb([128, T], F32, "sd")
        self.rstd = P.sb([128, T], F32, "rstd")
        self.alt = 0

    def next_ps(self):
        b = self.lin_ps[self.pi % len(self.lin_ps)]
        self.pi += 1
        return b

    def ew_eng(self):
        self.alt += 1
        return "dve" if self.alt % 2 else "pool"


def prep_weight(C, W_dram, K, N, scratch, gain_sb=None, dst_fn=None):
    P = C.P
    KC = (K + 127) // 128
    for kc in range(KC):
        rows = min(128, K - kc * 128)
        i = C.prep_i % 2
        C.prep_i += 1
        f, b = C.prep_f[i], C.prep_b[i]
        src = W_dram[kc * 128:kc * 128 + rows, :]
        P.dma_("sp", f[:rows, :N], src, [], [f])
        ceng = "pool" if kc % 2 == 0 else "dve"
        if gain_sb is not None:
            g = gain_sb[:rows, kc:kc + 1]
            P.op(ceng, lambda e, b=b, f=f, g=g, rows=rows: e.tensor_scalar(
                b[:rows, :N], f[:rows, :N], g, None, ALU.mult), [f, gain_sb], [b])
        else:
            P.op(ceng, lambda e, b=b, f=f, rows=rows: e.tensor_copy(b[:rows, :N], f[:rows, :N]), [f], [b])
        if dst_fn is None:
            dst = scratch[kc * 128:kc * 128 + rows, :]
            srcap = b[:rows, :N]
        else:
            dst, srcap = dst_fn(kc, rows, b)
        P.dma_("act", dst, srcap, [b], [scratch])


def linear(C, scratch, K, X, n0, n1, epi, Tn=T):
    P = C.P
    KC = (K + 127) // 128
    sv = scratch.t.ap().rearrange("(kc p) n -> p kc n", p=128) if K % 128 == 0 else None
    for g0 in range(n0, n1, 512):
        gw = min(512, n1 - g0)
        wt = C.wbuf[C.wi % 2]
        C.wi += 1
        if sv is not None:
            P.dma_("sp", wt[:, :KC, :gw], sv[:, :, g0:g0 + gw], [scratch], [wt])
        else:
            for kc in range(KC):
                rows = min(128, K - kc * 128)
                P.dma_("sp", wt[:rows, kc, :gw], scratch[kc * 128:kc * 128 + rows, g0:g0 + gw], [scratch], [wt])
        for m in range(0, gw, 128):
            mw = min(128, gw - m)
            ps = C.next_ps()
            for kc in range(KC):
                rows = min(128, K - kc * 128)
                P.mm(ps[:mw, :Tn], wt[:rows, kc, m:m + mw], X[:rows, kc, :Tn], kc == 0, kc == KC - 1,
                     [wt, X], [ps])
            epi(g0 + m, mw, ps)


def rms_scale(C, h, KC, Dn, xn, Tn=T):
    P = C.P
    sq, sd, rstd, ps_n = C.sq, C.sd, C.rstd, C.ps_n
    P.op("act", lambda e: e.activation(sq[:, :KC, :Tn], h[:, :KC, :Tn], AF.Square), [h], [sq])
    for kc in range(KC):
        P.mm(ps_n[:, :Tn], C.ones_bf[:, :], sq[:, kc, :Tn], kc == 0, kc == KC - 1, [sq, C.ones_bf], [ps_n])
    P.op("act", lambda e: e.activation(sd[:, :Tn], ps_n[:, :Tn], AF.Sqrt, bias=C.eps_ap, scale=1.0 / Dn),
         [ps_n, C.eps_b], [sd])
    P.op("dve", lambda e: e.reciprocal(rstd[:, :Tn], sd[:, :Tn]), [sd], [rstd])
    for kc in range(KC):
        eng = C.ew_eng()
        P.op(eng, lambda e, kc=kc: e.tensor_tensor(xn[:, kc, :Tn], h[:, kc, :Tn], rstd[:, :Tn], ALU.mult),
             [h, rstd], [xn])


def make_consts(C):
    P = C.P
    C.eps_b = P.sb([128, 1], F32, "eps")
    P.op("pool", lambda e: e.memset(C.eps_b[:, :], EPS), [], [C.eps_b])
    C.eps_ap = C.eps_b[:, 0:1]


def ffn(C, h, xn, a, sg, wgu_s, wd_s):
    P = C.P

    def epi_gu(n, mw, ps):
        q = n // 256
        if (n // 128) % 2 == 0:
            s = sg[q % 2]
            P.op("act", lambda e: e.activation(s[:, :], ps[:, :], AF.Silu), [ps], [s])
        else:
            s = sg[q % 2]
            P.op("dve", lambda e: e.tensor_tensor(a[:, q, :], s[:, :], ps[:, :], ALU.mult), [s, ps], [a])

    linear(C, wgu_s, D, xn, 0, 2 * DFF, epi_gu)

    def epi_d(n, mw, ps):
        m = n // 128
        P.op("dve", lambda e: e.scalar_tensor_tensor(h[:, m, :], ps[:, :], 0.5, h[:, m, :], ALU.mult, ALU.add),
             [ps, h], [h])

    linear(C, wd_s, DFF, a, 0, D, epi_d)


def prep_ffn(C, wg, wu, wd, gain_sb, wgu_s, wd_s):
    def dst_g(half):
        def f(kc, rows, b):
            dst = wgu_s.t.ap()[kc * 128:kc * 128 + rows, :].rearrange("p (q two c) -> p q two c", two=2, c=128)[:, :, half, :]
            src = b[:rows, :DFF].rearrange("p (q c) -> p q c", c=128)
            return dst, src
        return f
    prep_weight(C, wg, D, DFF, wgu_s, gain_sb, dst_g(0))
    prep_weight(C, wu, D, DFF, wgu_s, gain_sb, dst_g(1))
    prep_weight(C, wd, DFF, D, wd_s, None)


def build_phaseA(nc, ntok=4096):
    P = Prog(nc)
    hT = nc.dram_tensor("hT", [D, ntok], F32, kind="ExternalInput").ap()
    g1 = nc.dram_tensor("g1", [128, 8], F32, kind="ExternalInput").ap()
    gm = nc.dram_tensor("gm", [128, 8], F32, kind="ExternalInput").ap()
    wg = nc.dram_tensor("wg", [D, DFF], F32, kind="ExternalInput").ap()
    wu = nc.dram_tensor("wu", [D, DFF], F32, kind="ExternalInput").ap()
    wd = nc.dram_tensor("wd", [DFF, D], F32, kind="ExternalInput").ap()
    win = nc.dram_tensor("win", [D, NU], F32, kind="ExternalInput").ap()
    hT_o = nc.dram_tensor("hT_o", [D, ntok], F32, kind="ExternalOutput").ap()
    uT_o = nc.dram_tensor("uT_o", [NU, ntok], F32, kind="ExternalOutput").ap()
    C = Ctx(P)
    make_consts(C)
    wgu_s = P.dram("wgu_s", [D, 2 * DFF], BF16)
    wd_s = P.dram("wd_s", [DFF, D], BF16)
    win_s = P.dram("win_s", [D, NU], BF16)
    g1_sb = P.sb([128, 8], F32, "g1sb")
    gm_sb = P.sb([128, 8], F32, "gmsb")
    P.dma_("sp", g1_sb[:, :], g1, [], [g1_sb])
    P.dma_("sp", gm_sb[:, :], gm, [], [gm_sb])
    prep_ffn(C, wg, wu, wd, g1_sb, wgu_s, wd_s)
    prep_weight(C, win, D, NU, win_s, gm_sb)
    hb = [P.sb([128, 8, T], F32, f"h{i}") for i in range(2)]
    xn = [P.sb([128, 8, T], BF16, f"xn{i}") for i in range(2)]
    a = P.sb([128, 22, T], BF16, "a")
    sg = [P.sb([128, T], F32, f"sg{i}") for i in range(2)]
    stage = [P.sb([128, T], F32, f"stage{i}") for i in range(4)]
    hv = hT.rearrange("(kc p) t -> p kc t", p=128)
    hov = hT_o.rearrange("(kc p) t -> p kc t", p=128)
    nt = ntok // T
    sti = [0]
    for it in range(nt):
        h = hb[it % 2]
        x1 = xn[0]
        x2 = xn[1]
        P.dma_("sp", h[:, :, :], hv[:, :, it * T:(it + 1) * T], [], [h])
        rms_scale(C, h, 8, D, x1)
        ffn(C, h, x1, a, sg, wgu_s, wd_s)
        P.dma_("sp", hov[:, :, it * T:(it + 1) * T], h[:, :, :], [h], [])
        rms_scale(C, h, 8, D, x2)

        def epi_u(n, mw, ps):
            s = stage[sti[0] % 4]
            sti[0] += 1
            eng = "act" if sti[0] % 2 else "dve"
            if eng == "act":
                P.op("act", lambda e: e.activation(s[:mw, :], ps[:mw, :], AF.Copy), [ps], [s])
            else:
                P.op("dve", lambda e: e.tensor_copy(s[:mw, :], ps[:mw, :]), [ps], [s])
            P.dma_("sp", uT_o[n:n + mw, it * T:(it + 1) * T], s[:mw, :], [s], [])

        linear(C, win_s, D, x2, 0, NU, epi_u)
    P.emit()
    return nc


def build_mla(nc, S=8192):
    import math
    P = Prog(nc)
    NT = S // T
    cqT = nc.dram_tensor("cqT", [384, S], F32, kind="ExternalInput").ap()
    ckvT = nc.dram_tensor("ckvT", [256, S], F32, kind="ExternalInput").ap()
    krT = nc.dram_tensor("krT", [128, S], F32, kind="ExternalInput").ap()
    pos = nc.dram_tensor("pos", [1, S], I32, kind="ExternalInput").ap()
    wq = nc.dram_tensor("wq", [384, 512], F32, kind="ExternalInput").ap()
    wkv = nc.dram_tensor("wkv", [256, 512], F32, kind="ExternalInput").ap()
    gq = nc.dram_tensor("gq", [128, 3], F32, kind="ExternalInput").ap()
    gkv = nc.dram_tensor("gkv", [128, 2], F32, kind="ExternalInput").ap()
    rc = nc.dram_tensor("rc", [64, 2], F32, kind="ExternalInput").ap()
    yT = nc.dram_tensor("yT", [256, S], F32, kind="ExternalOutput").ap()
    C = Ctx(P, wbuf_kc=1, prep_n=512)
    make_consts(C)
    scale = 192.0 ** -0.5
    gq_sb = P.sb([128, 3], F32, "gq_sb")
    gkv_sb = P.sb([128, 2], F32, "gkv_sb")
    rc_sb = P.sb([64, 2], F32, "rc_sb")
    P.dma_("sp", gq_sb[:, :], gq, [], [gq_sb])
    P.dma_("sp", gkv_sb[:, :], gkv, [], [gkv_sb])
    P.dma_("sp", rc_sb[:, :], rc, [], [rc_sb])
    wq_sb = P.sb([128, 3, 512], BF16, "wq_sb")
    wkv_sb = P.sb([128, 2, 512], BF16, "wkv_sb")
    for (wdr, KC, g, dst) in ((wq, 3, gq_sb, wq_sb), (wkv, 2, gkv_sb, wkv_sb)):
        for kc in range(KC):
            f = C.prep_f[kc % 2]
            P.dma_("sp", f[:, :512], wdr[kc * 128:(kc + 1) * 128, :], [], [f])
            P.op("pool", lambda e, f=f, dst=dst, g=g, kc=kc: e.tensor_scalar(
                dst[:, kc, :], f[:, :512], g[:, kc:kc + 1], None, ALU.mult), [f, g], [dst])
    knT = [P.sb([128, S], BF16, f"knT{h}") for h in range(2)]
    kra = P.sb([128, S], BF16, "kra")
    V = P.sb([128, S // 128, 256], BF16, "V")
    kmax2 = [P.sb([128, 1], F32, f"kmax2_{h}") for h in range(2)]
    mxall = [P.sb([128, NT], F32, f"mxall{h}") for h in range(2)]
    P.op("pool", lambda e: e.memset(kra[:, :], 0.0), [], [kra])
    P.op("pool", lambda e: e.memset(kra[64:65, :], 1.0), [], [kra])
    cq_t = P.sb([128, 3, T], F32, "cq_t")
    ckv_t = P.sb([128, 2, T], F32, "ckv_t")
    kr_t = P.sb([64, T], F32, "kr_t")
    krp_t = P.sb([64, T], F32, "krp_t")
    pos_i = P.sb([64, T], I32, "pos_i")
    posf = P.sb([64, T], F32, "posf")
    ang = P.sb([64, T], F32, "ang")
    ki = P.sb([64, T], I32, "ki")
    kf = P.sb([64, T], F32, "kf")
    cos_t = P.sb([64, T], F32, "cos_t")
    sin_t = P.sb([64, T], F32, "sin_t")
    cqn = P.sb([128, 3, T], BF16, "cqn")
    ckvn = P.sb([128, 2, T], BF16, "ckvn")
    t1 = P.sb([64, T], F32, "t1")
    t2 = P.sb([64, T], F32, "t2")
    sqk = P.sb([128, T], BF16, "sqk")
    sqr = P.sb([128, T], BF16, "sqr")
    P.op("pool", lambda e: e.memset(sqr[:, :], 0.0), [], [sqr])
    mx = P.sb([128, 1], F32, "mx")
    ps_a = C.lin_ps[0]
    ps_b = C.lin_ps[1]
    ps_s = [C.lin_ps[2], C.lin_ps[3]]
    ps_o = C.lin_ps[4]
    ps_l = C.lin_ps[5]
    ps_n = C.ps_n
    TWO_PI = 2.0 * math.pi

    def rope_tables(it):
        P.dma_("sp", pos_i[:, :], pos[0:1, it * T:(it + 1) * T].partition_broadcast(64), [], [pos_i])
        P.op("dve", lambda e: e.tensor_copy(posf[:, :], pos_i[:, :]), [pos_i], [posf])
        P.op("dve", lambda e: e.tensor_scalar(ang[:, :], posf[:, :], rc_sb[:, 0:1], None, ALU.mult), [posf, rc_sb], [ang])
        P.op("dve", lambda e: e.tensor_scalar(ki[:, :], ang[:, :], 1.0 / TWO_PI, None, ALU.mult), [ang], [ki])
        P.op("dve", lambda e: e.tensor_copy(kf[:, :], ki[:, :]), [ki], [kf])
        P.op("dve", lambda e: e.scalar_tensor_tensor(t1[:, :], kf[:, :], -TWO_PI, ang[:, :], ALU.mult, ALU.add), [kf, ang], [t1])

        def wrap(r, hi=True, lo=True):
            if hi:
                P.op("dve", lambda e: e.tensor_scalar(t2[:, :], r[:, :], math.pi, -TWO_PI, ALU.is_gt, ALU.mult), [r], [t2])
                P.op("dve", lambda e: e.tensor_tensor(r[:, :], r[:, :], t2[:, :], ALU.add), [r, t2], [r])
            if lo:
                P.op("dve", lambda e: e.tensor_scalar(t2[:, :], r[:, :], -math.pi, TWO_PI, ALU.is_lt, ALU.mult), [r], [t2])
                P.op("dve", lambda e: e.tensor_tensor(r[:, :], r[:, :], t2[:, :], ALU.add), [r, t2], [r])
        wrap(t1)
        P.op("act", lambda e: e.activation(sin_t[:, :], t1[:, :], AF.Sin, scale=rc_sb[:, 1:2]), [t1, rc_sb], [sin_t])
        P.op("dve", lambda e: e.tensor_scalar(t1[:, :], t1[:, :], 0.5 * math.pi, None, ALU.add), [t1], [t1])
        wrap(t1, lo=False)
        P.op("act", lambda e: e.activation(cos_t[:, :], t1[:, :], AF.Sin), [t1], [cos_t])

    ckvv = ckvT.rearrange("(kc p) t -> p kc t", p=128)
    for it in range(NT):
        sl = slice(it * T, (it + 1) * T)
        P.dma_("sp", ckv_t[:, :, :], ckvv[:, :, sl], [], [ckv_t])
        P.dma_("sp", kr_t[:, :], krT[0:64, sl], [], [kr_t])
        P.dma_("sp", krp_t[:, :], krT[64:128, sl], [], [krp_t])
        rope_tables(it)
        rms_scale(C, ckv_t, 2, 256, ckvn)
        P.op("dve", lambda e: e.tensor_tensor(t1[:, :], kr_t[:, :], cos_t[:, :], ALU.mult), [kr_t, cos_t], [t1])
        P.op("pool", lambda e: e.tensor_tensor(t2[:, :], krp_t[:, :], sin_t[:, :], ALU.mult), [krp_t, sin_t], [t2])
        P.op("dve", lambda e, sl=sl: e.tensor_tensor(kra[0:64, sl], t1[:, :], t2[:, :], ALU.add), [t1, t2], [kra])
        P.op("act", lambda e, sl=sl: e.activation(sqr[0:64, :], kra[0:64, sl], AF.Square), [kra], [sqr])
        for h in range(2):
            ps = ps_a if h == 0 else ps_b
            for kc in range(2):
                P.mm(ps[:, :], wkv_sb[:, kc, h * 128:(h + 1) * 128], ckvn[:, kc, :], kc == 0, kc == 1, [wkv_sb, ckvn], [ps])
            P.op("dve", lambda e, ps=ps, h=h, sl=sl: e.tensor_copy(knT[h][:, sl], ps[:, :]), [ps], [knT[h]])
            P.op("dve", lambda e, h=h, sl=sl: e.tensor_tensor(sqk[:, :], knT[h][:, sl], knT[h][:, sl], ALU.mult), [knT[h]], [sqk])
            P.mm(ps_n[:, :], C.ones_bf[:, :], sqk[:, :], True, False, [C.ones_bf, sqk], [ps_n])
            P.mm(ps_n[:, :], C.ones_bf[:, :], sqr[:, :], False, True, [C.ones_bf, sqr], [ps_n])
            P.op("dve", lambda e, h=h, it=it: e.tensor_reduce(mxall[h][:, it:it + 1], ps_n[:, :], AX.X, ALU.max), [ps_n], [mxall[h]])
        for blk in range(4):
            ps = C.lin_ps[2 + blk % 2]
            for kc in range(2):
                P.mm(ps[:, :256], ckvn[:, kc, blk * 128:(blk + 1) * 128], wkv_sb[:, kc, 256:512], kc == 0, kc == 1,
                     [ckvn, wkv_sb], [ps])
            P.op("act", lambda e, ps=ps, blk=blk, it=it: e.activation(V[:, it * 4 + blk, :], ps[:, :256], AF.Copy), [ps], [V])
    negK = [P.sb([128, 1], F32, f"negK{h}") for h in range(2)]
    for h in range(2):
        P.op("dve", lambda e, h=h: e.tensor_reduce(kmax2[h][:, :], mxall[h][:, :], AX.X, ALU.max), [mxall[h]], [kmax2[h]])
        P.op("act", lambda e, h=h: e.activation(negK[h][:, :], kmax2[h][:, :], AF.Sqrt), [kmax2[h]], [negK[h]])
        P.op("dve", lambda e, h=h: e.tensor_scalar(negK[h][:, :], negK[h][:, :], -1.0, None, ALU.mult), [negK[h]], [negK[h]])
    cqv = cqT.rearrange("(kc p) t -> p kc t", p=128)
    qn = [P.sb([128, T], BF16, f"qn{h}") for h in range(2)]
    qra = [P.sb([128, T], BF16, f"qra{h}") for h in range(2)]
    for h in range(2):
        P.op("pool", lambda e, h=h: e.memset(qra[h][:, :], 0.0), [], [qra[h]])
    qnorm = P.sb([128, T], F32, "qnorm")
    pT = [P.sb([128, T], BF16, f"pT{i}") for i in range(3)]
    rec = P.sb([128, T], F32, "rec")
    ost = [P.sb([128, T], F32, f"ost{i}") for i in range(2)]
    pti = 0
    for it in range(NT):
        sl = slice(it * T, (it + 1) * T)
        P.dma_("sp", cq_t[:, :, :], cqv[:, :, sl], [], [cq_t])
        rope_tables(it)
        rms_scale(C, cq_t, 3, 384, cqn)
        for h in range(2):
            base = h * 256
            for kc in range(3):
                P.mm(ps_a[:, :], wq_sb[:, kc, base:base + 128], cqn[:, kc, :], kc == 0, kc == 2, [wq_sb, cqn], [ps_a])
            P.op("dve", lambda e, h=h: e.tensor_copy(qn[h][:, :], ps_a[:, :]), [ps_a], [qn[h]])
            P.op("dve", lambda e, h=h: e.tensor_tensor(sqk[:, :], qn[h][:, :], qn[h][:, :], ALU.mult), [qn[h]], [sqk])
            for kc in range(3):
                P.mm(ps_b[0:64, :], wq_sb[:, kc, base + 128:base + 192], cqn[:, kc, :], kc == 0, kc == 2, [wq_sb, cqn], [ps_b])
            P.op("dve", lambda e: e.tensor_tensor(t1[:, :], ps_b[0:64, :], cos_t[:, :], ALU.mult), [ps_b, cos_t], [t1])
            for kc in range(3):
                P.mm(ps_b[0:64, :], wq_sb[:, kc, base + 192:base + 256], cqn[:, kc, :], kc == 0, kc == 2, [wq_sb, cqn], [ps_b])
            P.op("dve", lambda e: e.tensor_tensor(t2[:, :], ps_b[0:64, :], sin_t[:, :], ALU.mult), [ps_b, sin_t], [t2])
            P.op("dve", lambda e, h=h: e.tensor_tensor(qra[h][0:64, :], t1[:, :], t2[:, :], ALU.add), [t1, t2], [qra[h]])
            P.op("act", lambda e, h=h: e.activation(sqr[0:64, :], qra[h][0:64, :], AF.Square), [qra[h]], [sqr])
            P.mm(ps_n[:, :], C.ones_bf[:, :], sqk[:, :], True, False, [C.ones_bf, sqk], [ps_n])
            P.mm(ps_n[:, :], C.ones_bf[:, :], sqr[:, :], False, True, [C.ones_bf, sqr], [ps_n])
            P.op("act", lambda e: e.activation(qnorm[:, :], ps_n[:, :], AF.Sqrt), [ps_n], [qnorm])
            P.op("dve", lambda e, h=h: e.tensor_scalar(qra[h][64:65, :], qnorm[64:65, :], negK[h][64:65, 0:1], None, ALU.mult),
                 [qnorm, negK[h]], [qra[h]])
        for h in range(2):
            NK = S // 128

            def s_mm(kc, h=h):
                ks = slice(kc * 128, (kc + 1) * 128)
                pss = ps_s[kc % 2]
                P.mm(pss[:, :], knT[h][:, ks], qn[h][:, :], True, False, [knT[h], qn[h]], [pss])
                P.mm(pss[:, :], kra[:, ks], qra[h][:, :], False, True, [kra, qra[h]], [pss])
            s_mm(0)
            for kc in range(NK):
                if kc + 1 < NK:
                    s_mm(kc + 1)
                pss = ps_s[kc % 2]
                p = pT[pti % 3]
                pti += 1
                P.op("act", lambda e, p=p, pss=pss: e.activation(p[:, :], pss[:, :], AF.Exp, scale=scale), [pss], [p])
                P.mm(ps_o[:, :], V[:, kc, h * 128:(h + 1) * 128], p[:, :], kc == 0, kc == NK - 1, [V, p], [ps_o])
                P.mm(ps_l[:, :], C.ones_bf[:, :], p[:, :], kc == 0, kc == NK - 1, [C.ones_bf, p], [ps_l])
            o = ost[h]
            P.op("dve", lambda e: e.reciprocal(rec[:, :], ps_l[:, :]), [ps_l], [rec])
            P.op("dve", lambda e, o=o: e.tensor_tensor(o[:, :], ps_o[:, :], rec[:, :], ALU.mult), [ps_o, rec], [o])
            P.dma_("sp", yT[h * 128:(h + 1) * 128, sl], o[:, :], [o], [])
    P.emit()
    return nc


def rms_scale_g(C, h, KC, Dn, out, gain, Tn=T):
    P = C.P
    sq, sd, rstd, ps_n = C.sq, C.sd, C.rstd, C.ps_n
    P.op("act", lambda e: e.activation(sq[:, :KC, :Tn], h[:, :KC, :Tn], AF.Square), [h], [sq])
    for kc in range(KC):
        P.mm(ps_n[:, :Tn], C.ones_bf[:, :], sq[:, kc, :Tn], kc == 0, kc == KC - 1, [sq, C.ones_bf], [ps_n])
    P.op("act", lambda e: e.activation(sd[:, :Tn], ps_n[:, :Tn], AF.Sqrt, bias=C.eps_ap, scale=1.0 / Dn),
         [ps_n, C.eps_b], [sd])
    P.op("dve", lambda e: e.reciprocal(rstd[:, :Tn], sd[:, :Tn]), [sd], [rstd])
    for kc in range(KC):
        P.op("dve", lambda e, kc=kc: e.scalar_tensor_tensor(out[:, kc, :Tn], h[:, kc, :Tn], gain[:, kc:kc + 1],
                                                             rstd[:, :Tn], ALU.mult, ALU.mult), [h, rstd, gain], [out])


def build_phaseC(nc, ntok=4096, last=False):
    P = Prog(nc)
    def inp(name, shape):
        return nc.dram_tensor(name, shape, F32, kind="ExternalInput").ap()
    hT = inp("hT", [D, ntok]); yT = inp("yT", [D, ntok]); pT = inp("pT", [256, ntok])
    wout = inp("wout", [D, D]); g2 = inp("g2", [128, 8]); wg = inp("wg", [D, DFF]); wu = inp("wu", [D, DFF])
    wd = inp("wd", [DFF, D]); gg = inp("gg", [128, 8]); wgate = inp("wgate", [D, D]); wproj = inp("wproj", [256, D])
    gp = inp("gp", [128, 8]); gf = inp("gf", [128, 8])
    hT_o = nc.dram_tensor("hT_o", [D, ntok], F32, kind="ExternalOutput").ap()
    C = Ctx(P)
    make_consts(C)
    wout_s = P.dram("wout_s", [D, D], BF16)
    wgu_s = P.dram("wgu_s", [D, 2 * DFF], BF16)
    wd_s = P.dram("wd_s", [DFF, D], BF16)
    wgate_s = P.dram("wgate_s", [D, D], BF16)
    wproj_s = P.dram("wproj_s", [256, D], BF16)
    gs = {}
    for nm, ap in (("g2", g2), ("gg", gg), ("gp", gp), ("gf", gf)):
        gs[nm] = P.sb([128, 8], F32, nm + "_sb")
        P.dma_("sp", gs[nm][:, :], ap, [], [gs[nm]])
    prep_weight(C, wout, D, D, wout_s, None)
    prep_ffn(C, wg, wu, wd, gs["g2"], wgu_s, wd_s)
    prep_weight(C, wgate, D, D, wgate_s, gs["gg"])
    prep_weight(C, wproj, 256, D, wproj_s, None)
    hb = [P.sb([128, 8, T], F32, f"h{i}") for i in range(2)]
    yb32 = P.sb([128, 8, T], F32, "y32")
    xb = P.sb([128, 8, T], BF16, "xb")
    pt32 = P.sb([128, 2, T], F32, "pt32")
    ptb = P.sb([128, 2, T], BF16, "ptb")
    a = P.sb([128, 22, T], BF16, "a")
    sg = [P.sb([128, T], F32, f"sg{i}") for i in range(2)]
    hv = hT.rearrange("(kc p) t -> p kc t", p=128)
    yv = yT.rearrange("(kc p) t -> p kc t", p=128)
    pv = pT.rearrange("(kc p) t -> p kc t", p=128)
    hov = hT_o.rearrange("(kc p) t -> p kc t", p=128)
    for it in range(ntok // T):
        sl = slice(it * T, (it + 1) * T)
        h = hb[it % 2]
        P.dma_("sp", h[:, :, :], hv[:, :, sl], [], [h])
        P.dma_("sp", yb32[:, :, :], yv[:, :, sl], [], [yb32])
        P.dma_("sp", pt32[:, :, :], pv[:, :, sl], [], [pt32])
        for kc in range(8):
            P.op(C.ew_eng(), lambda e, kc=kc: e.tensor_copy(xb[:, kc, :], yb32[:, kc, :]), [yb32], [xb])
        P.op("pool", lambda e: e.tensor_copy(ptb[:, :, :], pt32[:, :, :]), [pt32], [ptb])

        def epi_o(n, mw, ps, h=h):
            m = n // 128
            P.op("dve", lambda e: e.tensor_tensor(h[:, m, :], h[:, m, :], ps[:, :], ALU.add), [ps, h], [h])
        linear(C, wout_s, D, xb, 0, D, epi_o)
        rms_scale(C, h, 8, D, xb)
        ffn(C, h, xb, a, sg, wgu_s, wd_s)
        def epi_p(n, mw, ps):
            m = n // 128
            P.op("act", lambda e: e.activation(yb32[:, m, :], ps[:, :], AF.Copy), [ps], [yb32])
        linear(C, wproj_s, 256, ptb, 0, D, epi_p)
        rms_scale_g(C, yb32, 8, D, yb32, gs["gp"])
        rms_scale(C, h, 8, D, xb)

        def epi_g(n, mw, ps, h=h):
            m = n // 128
            s = sg[m % 2]
            P.op("act", lambda e: e.activation(s[:, :], ps[:, :], AF.Sigmoid), [ps], [s])
            P.op("pool", lambda e: e.tensor_tensor(s[:, :], s[:, :], yb32[:, m, :], ALU.mult), [s, yb32], [s])
            P.op("dve", lambda e: e.tensor_tensor(h[:, m, :], h[:, m, :], s[:, :], ALU.add), [s, h], [h])
        linear(C, wgate_s, D, xb, 0, D, epi_g)
        if last:
            rms_scale_g(C, h, 8, D, h, gs["gf"])
        P.dma_("sp", hov[:, :, sl], h[:, :, :], [h], [])
    P.emit()
    return nc


def fnet_consts():
    N = 8192
    k1 = np.arange(128)
    s1 = np.arange(128)
    a1 = 2 * np.pi * np.outer(s1, k1) / 128.0
    F1 = np.concatenate([np.cos(a1), -np.sin(a1)], 1).astype(np.float32)
    s2 = np.arange(64)
    at = 2 * np.pi * np.outer(s2, k1) / N
    TW = np.concatenate([np.cos(at), np.sin(at)], 1).reshape(1, 64 * 256).astype(np.float32)
    ch = np.arange(64)
    ac = 2 * np.pi * np.outer(ch, ch) / 64.0
    nrm = 1.0 / np.sqrt(N * 64.0)
    Cc, Sc = np.cos(ac) * nrm, np.sin(ac) * nrm
    Z = np.zeros((64, 64))
    CcBD = np.block([[Cc, Z], [Z, Cc]]).astype(np.float32)
    ScBD = np.block([[Sc, Z], [Z, Sc]]).astype(np.float32)
    a4 = 2 * np.pi * np.outer(s2, np.arange(64)) / 64.0
    C4 = np.concatenate([np.cos(a4), np.sin(a4)], 1).astype(np.float32)
    return dict(F1=F1, TW=TW, CcBD=CcBD, ScBD=ScBD, C4=C4)


def build_fnet(nc):
    P = Prog(nc)
    def inp(name, shape):
        return nc.dram_tensor(name, shape, F32, kind="ExternalInput").ap()
    Z1 = inp("Z1", [128, 64, 128]); F1 = inp("F1", [128, 256]); TW = inp("TW", [1, 64 * 256])
    CcBD = inp("CcBD", [128, 128]); ScBD = inp("ScBD", [128, 128]); WBD = inp("WBD", [128, 128])
    C4 = inp("C4", [64, 128]); fb = inp("fb", [128, 1])
    yT = nc.dram_tensor("yT", [128, 8192], F32, kind="ExternalOutput").ap()
    f1 = P.sb([128, 256], F32, "f1"); tw = P.sb([128, 64, 256], F32, "tw")
    cc = P.sb([128, 128], F32, "cc"); sc = P.sb([128, 128], F32, "sc"); wbd = P.sb([128, 128], F32, "wbd")
    c4 = P.sb([64, 128], F32, "c4"); fb_sb = P.sb([128, 1], F32, "fb_sb")
    P.dma_("sp", f1[:, :], F1, [], [f1])
    P.dma_("sp", tw[:, :, :].rearrange("p a b -> p (a b)"), TW[0:1, :].partition_broadcast(128), [], [tw])
    for (t, a) in ((cc, CcBD), (sc, ScBD), (wbd, WBD), (c4, C4), (fb_sb, fb)):
        P.dma_("sp", t[:, :], a, [], [t])
    pss = [P.ps([128, 512], F32, f"fps{i}") for i in range(8)]
    R1 = P.sb([128, 256], F32, "R1"); R2 = P.sb([128, 256], F32, "R2")
    P.mm(pss[0][:, 0:128], cc[:, :], wbd[:, :], True, True, [cc, wbd], [pss[0]])
    P.mm(pss[1][:, 0:128], sc[:, :], wbd[:, :], True, True, [sc, wbd], [pss[1]])
    P.op("dve", lambda e: e.tensor_copy(R1[:, 0:128], pss[0][:, 0:128]), [pss[0]], [R1])
    P.op("dve", lambda e: e.tensor_copy(R2[:, 128:256], pss[0][:, 0:128]), [pss[0]], [R2])
    P.op("dve", lambda e: e.tensor_copy(R2[:, 0:128], pss[1][:, 0:128]), [pss[1]], [R2])
    P.op("dve", lambda e: e.tensor_scalar(R1[:, 128:256], pss[1][:, 0:128], -1.0, None, ALU.mult), [pss[1]], [R1])
    Tre = P.sb([128, 64, 128], F32, "Tre"); Tim = P.sb([128, 64, 128], F32, "Tim")
    zb = [P.sb([128, 128], F32, f"zb{i}") for i in range(3)]
    tt = [[P.sb([128, 128], F32, f"tt{i}_{j}") for j in range(4)] for i in range(2)]
    for s2 in range(64):
        z = zb[s2 % 3]
        P.dma_("sp", z[:, :], Z1[:, s2, :], [], [z])
        ps = pss[2 + s2 % 2]
        P.mm(ps[:, 0:256], z[:, :], f1[:, :], True, True, [z, f1], [ps])
        t = tt[s2 % 2]
        P.op("dve", lambda e, t=t, ps=ps, s2=s2: e.tensor_tensor(t[0][:, :], ps[:, 0:128], tw[:, s2, 0:128], ALU.mult), [ps, tw], [t[0]])
        P.op("dve", lambda e, t=t, ps=ps, s2=s2: e.tensor_tensor(t[1][:, :], ps[:, 128:256], tw[:, s2, 128:256], ALU.mult), [ps, tw], [t[1]])
        P.op("dve", lambda e, t=t, ps=ps, s2=s2: e.tensor_tensor(t[2][:, :], ps[:, 128:256], tw[:, s2, 0:128], ALU.mult), [ps, tw], [t[2]])
        P.op("dve", lambda e, t=t, ps=ps, s2=s2: e.tensor_tensor(t[3][:, :], ps[:, 0:128], tw[:, s2, 128:256], ALU.mult), [ps, tw], [t[3]])
        P.op("pool", lambda e, t=t, s2=s2: e.tensor_tensor(Tre[:, s2, :], t[0][:, :], t[1][:, :], ALU.add), [t[0], t[1]], [Tre])
        P.op("pool", lambda e, t=t, s2=s2: e.tensor_tensor(Tim[:, s2, :], t[2][:, :], t[3][:, :], ALU.subtract), [t[2], t[3]], [Tim])
    ysb = P.sb([128, 64, 128], F32, "ysb")
    pq = [P.sb([64, 256], F32, f"pq{i}") for i in range(3)]
    for blk in range(16):
        psy = pss[6 + blk % 2]
        for j in range(8):
            k1 = blk * 8 + j
            ps = pss[2 + k1 % 4]
            P.mm(ps[0:64, 0:256], Tre[:, :, k1], R1[:, :], True, False, [Tre, R1], [ps])
            P.mm(ps[0:64, 0:256], Tim[:, :, k1], R2[:, :], False, True, [Tim, R2], [ps])
            q = pq[k1 % 3]
            if k1 % 2 == 0:
                P.op("act", lambda e, q=q, ps=ps: e.activation(q[:, :], ps[0:64, 0:256], AF.Copy), [ps], [q])
            else:
                P.op("dve", lambda e, q=q, ps=ps: e.tensor_copy(q[:, :], ps[0:64, 0:256]), [ps], [q])
            P.mm(psy[:, j * 64:(j + 1) * 64], q[:, 0:128], c4[:, 0:64], True, False, [q, c4], [psy])
            P.mm(psy[:, j * 64:(j + 1) * 64], q[:, 128:256], c4[:, 64:128], False, True, [q, c4], [psy])
        P.op("dve", lambda e, blk=blk, psy=psy: e.tensor_scalar(
            ysb[:, :, blk * 8:(blk + 1) * 8], psy[:, :].rearrange("e (j k) -> e k j", j=8), fb_sb[:, 0:1], None, ALU.add),
            [psy, fb_sb], [ysb])
    P.dma_("sp", yT, ysb[:, :, :].rearrange("e a b -> e (a b)"), [ysb], [])
    P.emit()
    return nc


def mlstm_consts():
    i = np.arange(128)
    triu = (i[:, None] <= i[None, :]).astype(np.float32)
    tril = (i[:, None] >= i[None, :]).astype(np.float32)
    return dict(TRI=np.ascontiguousarray(np.stack([triu, tril], 1).reshape(128, 256)),
                IDN=np.eye(128, dtype=np.float32))


def build_mlstm(nc, NCH=64):
    S = NCH * 128
    P = Prog(nc)
    def inp(name, shape):
        return nc.dram_tensor(name, shape, F32, kind="ExternalInput").ap()
    mxT = inp("mxT", [128, S]); moT = inp("moT", [128, S]); mv = inp("mv", [128, NCH, 128]); G = inp("G", [128, NCH, 8])
    gb = inp("gb", [128, 8]); cw = inp("cw", [128, 8])
    WqBD = inp("WqBD", [128, 256]); WkBD = inp("WkBD", [128, 128]); TRI = inp("TRI", [128, 256]); IDN = inp("IDN", [128, 128])
    yT = nc.dram_tensor("yT", [128, S], F32, kind="ExternalOutput").ap()
    bufA = P.sb([128, S + 4], F32, "bufA")
    xc = P.sb([128, S], F32, "xc")
    qTm = [P.sb([128, S], F32, f"qTm{h}") for h in range(2)]
    Hs = P.sb([128, NCH, 128], F32, "Hs")
    g_sb = P.sb([128, NCH, 8], F32, "g_sb"); gb_sb = P.sb([128, 8], F32, "gb_sb"); cw_sb = P.sb([128, 8], F32, "cw_sb")
    wq = P.sb([128, 256], F32, "wq"); wk = P.sb([128, 128], F32, "wk"); tri = P.sb([128, 256], F32, "tri")
    idn = P.sb([128, 128], F32, "idn"); ones = P.sb([128, 128], F32, "ones32"); one1 = P.sb([128, 1], F32, "one1")
    eps_b = P.sb([128, 1], F32, "eps_b")
    P.op("pool", lambda e: e.memset(ones[:, :], 1.0), [], [ones])
    P.op("pool", lambda e: e.memset(one1[:, :], 1.0), [], [one1])
    P.op("pool", lambda e: e.memset(eps_b[:, :], EPS), [], [eps_b])
    for (t, a) in ((g_sb, G), (gb_sb, gb), (cw_sb, cw), (wq, WqBD), (wk, WkBD), (tri, TRI), (idn, IDN)):
        if t is g_sb:
            P.dma_("sp", t[:, :, :], a, [], [t])
        else:
            P.dma_("sp", t[:, :], a, [], [t])
    pp = [P.ps([128, 512], F32, f"mps{i}") for i in range(8)]
    P.op("pool", lambda e: e.memset(bufA[:, 0:2], 0.0), [], [bufA])
    P.op("pool", lambda e: e.memset(bufA[:, S + 2:S + 4], 0.0), [], [bufA])
    P.dma_("sp", bufA[:, 2:S + 2], mxT, [], [bufA])
    HS = S // 2
    for half, eng in ((0, "dve"), (1, "dve")):
        o = half * HS
        xh = P.view(xc.t, f"xc_half{half}") if False else xc
        P.op(eng, lambda e, o=o: e.tensor_scalar(xc[:, o:o + HS], bufA[:, o:o + HS], cw_sb[:, 0:1], None, ALU.mult), [bufA, cw_sb], [xc])
        for j in range(1, 5):
            P.op(eng, lambda e, o=o, j=j: e.scalar_tensor_tensor(xc[:, o:o + HS], bufA[:, o + j:o + j + HS], cw_sb[:, j:j + 1],
                                                                    xc[:, o:o + HS], ALU.mult, ALU.add), [bufA, cw_sb, xc], [xc])
    P.op("act", lambda e: e.activation(xc[:, :], xc[:, :], AF.Silu, bias=cw_sb[:, 5:6]), [xc, cw_sb], [xc])
    kT = bufA
    for it in range(S // 512):
        sl = slice(it * 512, (it + 1) * 512)
        pk_ = pp[2 + it % 2]
        for hl in range(2):
            pq_ = pp[hl]
            P.mm(pq_[:, :], wq[:, hl * 128:(hl + 1) * 128], xc[:, sl], True, True, [wq, xc], [pq_])
            P.op("act", lambda e, pq_=pq_, sl=sl, hl=hl: e.activation(qTm[hl][:, sl], pq_[:, :], AF.Copy), [pq_], [qTm[hl]])
        P.mm(pk_[:, :], wk[:, :], xc[:, sl], True, True, [wk, xc], [pk_])
        P.op("dve", lambda e, pk_=pk_, sl=sl: e.tensor_copy(kT[:, sl], pk_[:, :]), [pk_], [kT])
    for col in range(8):
        P.op("dve", lambda e, col=col: e.tensor_scalar(g_sb[:, :, col], g_sb[:, :, col], gb_sb[:, col:col + 1], None, ALU.add),
             [g_sb, gb_sb], [g_sb])
    l1 = P.sb([128, NCH, 4], F32, "l1")
    P.op("act", lambda e: e.activation(l1[:, :, :], g_sb[:, :, 4:8], AF.Exp, scale=-1.0), [g_sb], [l1])
    P.op("act", lambda e: e.activation(l1[:, :, :], l1[:, :, :], AF.Ln, bias=one1[:, 0:1]), [l1, one1], [l1])
    l1d = [P.sb([128, NCH, 2], F32, f"l1d{d}") for d in range(2)]
    for d in range(2):
        P.op("dve", lambda e, d=d: e.tensor_copy(l1d[d][:, :, :], l1[:, :, d * 2:d * 2 + 2]), [l1], [l1d[d]])
    Bt = P.sb([128, NCH, 4], F32, "Bt"); Gbc = P.sb([128, NCH, 4], F32, "Gbc")
    psg = pp[4]
    for d in range(2):
        P.mm(psg[:, d * 2 * NCH:(d + 1) * 2 * NCH], tri[:, d * 128:(d + 1) * 128], l1d[d][:, :, :].rearrange("p c h -> p (c h)"),
             True, True, [tri, l1d[d]], [psg])
        P.mm(psg[:, 256 + d * 2 * NCH:256 + (d + 1) * 2 * NCH], ones[:, :], l1d[d][:, :, :].rearrange("p c h -> p (c h)"),
             True, True, [ones, l1d[d]], [psg])
    for d in range(2):
        P.op("dve", lambda e, d=d: e.tensor_scalar(Bt[:, :, d * 2:d * 2 + 2],
                                                    psg[:, d * 2 * NCH:(d + 1) * 2 * NCH].rearrange("p (c h) -> p c h", h=2), -1.0, None, ALU.mult),
             [psg], [Bt])
        P.op("dve", lambda e, d=d: e.tensor_scalar(Gbc[:, :, d * 2:d * 2 + 2],
                                                    psg[:, 256 + d * 2 * NCH:256 + (d + 1) * 2 * NCH].rearrange("p (c h) -> p c h", h=2), -1.0, None, ALU.mult),
             [psg], [Gbc])
    ek = P.sb([128, NCH, 4], F32, "ek"); eq = P.sb([128, NCH, 4], F32, "eq"); eg = P.sb([128, NCH, 4], F32, "eg")
    tg = P.sb([128, NCH, 4], F32, "tg")
    P.op("dve", lambda e: e.tensor_tensor(tg[:, :, :], Gbc[:, :, :], Bt[:, :, :], ALU.subtract), [Gbc, Bt], [tg])
    P.op("dve", lambda e: e.tensor_tensor(ek[:, :, :], tg[:, :, :], g_sb[:, :, 0:4], ALU.add), [tg, g_sb], [ek])
    P.op("act", lambda e: e.activation(ek[:, :, :], ek[:, :, :], AF.Exp), [ek], [ek])
    P.op("act", lambda e: e.activation(eq[:, :, :], tg[:, :, :], AF.Exp, scale=-1.0), [tg], [eq])
    P.op("dve", lambda e: e.tensor_scalar(eq[:, :, :], eq[:, :, :], 0.125, None, ALU.mult), [eq], [eq])
    P.op("act", lambda e: e.activation(eg[:, :, :], Gbc[:, :, :], AF.Exp), [Gbc], [eg])
    egcol = [P.sb([128, NCH], F32, f"egcol{d}") for d in range(2)]
    for d in range(2):
        for hl in range(2):
            P.op("dve", lambda e, d=d, hl=hl: e.tensor_copy(egcol[d][hl * 64:(hl + 1) * 64, :], eg[hl * 64:(hl + 1) * 64, :, d * 2 + hl]),
                 [eg], [egcol[d]])
    mask2 = [P.sb([128, 256], F32, f"mask2_{d}") for d in range(2)]
    for d in range(2):
        for hl in range(2):
            P.op("pool", lambda e, d=d, hl=hl: e.tensor_copy(mask2[d][:, hl * 128:(hl + 1) * 128], tri[:, d * 128:(d + 1) * 128]),
                 [tri], [mask2[d]])
    P.op("pool", lambda e: e.memset(Hs[:, :, :], 0.0), [], [Hs])
    Cst = [P.sb([128, 130], F32, f"Cst{d}") for d in range(2)]
    for d in range(2):
        P.op("pool", lambda e, d=d: e.memset(Cst[d][:, :], 0.0), [], [Cst[d]])
    mvb = [P.sb([128, 128], F32, f"mvb{i}") for i in range(4)]
    vpp = [P.sb([128, 130], F32, f"vpp{i}") for i in range(4)]
    ktok = [P.sb([128, 128], F32, f"ktok{i}") for i in range(4)]
    S0 = [P.sb([128, 256], F32, f"S0_{i}") for i in range(4)]
    sm = [[P.sb([128, 2], F32, f"sm{i}_{j}") for j in range(3)] for i in range(2)]
    psKt, psK, psN, psU = pp[0], pp[1], (pp[2], pp[3]), (pp[5], pp[6])
    step = 0
    for i in range(NCH):
        for d in range(2):
            c = i if d == 0 else NCH - 1 - i
            cn = c + 1 if d == 0 else c - 1
            cs = slice(c * 128, (c + 1) * 128)
            r = step % 4
            step += 1
            m_, v_, kt_, s0_ = mvb[r], vpp[r], ktok[r], S0[r]
            P.dma_("sp", m_[:, :], mv[:, c, :], [], [m_])
            for hl in range(2):
                col = d * 2 + hl
                P.op("pool", lambda e, hl=hl, col=col, c=c, v_=v_, m_=m_: e.tensor_scalar(
                    v_[:, hl * 65:hl * 65 + 64], m_[:, hl * 64:(hl + 1) * 64], ek[:, c, col:col + 1], None, ALU.mult), [m_, ek], [v_])
                P.op("pool", lambda e, hl=hl, col=col, c=c, v_=v_: e.tensor_copy(v_[:, hl * 65 + 64:hl * 65 + 65], ek[:, c, col:col + 1]),
                     [ek], [v_])
            P.mm(psKt[:, 0:128], xc[:, cs], wk[:, :], True, True, [xc, wk], [psKt])
            P.op("act", lambda e, kt_=kt_: e.activation(kt_[:, :], psKt[:, 0:128], AF.Copy), [psKt], [kt_])
            for hl in range(2):
                hs = slice(hl * 64, (hl + 1) * 64)
                P.mm(psK[:, hl * 128:(hl + 1) * 128], kT[:, cs], qTm[hl][:, cs], True, True, [kT, qTm[hl]], [psK])
            P.op("dve", lambda e, s0_=s0_, d=d: e.tensor_tensor(s0_[:, :], psK[:, 0:256], mask2[d][:, :], ALU.mult), [psK, mask2[d]], [s0_])
            pn = psN[d]
            for hl in range(2):
                hs = slice(hl * 64, (hl + 1) * 64)
                P.mm(pn[:, hl * 128:hl * 128 + 65], s0_[:, hl * 128:(hl + 1) * 128], v_[:, hl * 65:(hl + 1) * 65], True, False,
                     [s0_, v_], [pn])
                P.mm(pn[:, hl * 128:hl * 128 + 65], qTm[hl][:, cs], Cst[d][:, hl * 65:(hl + 1) * 65], False, True,
                     [qTm[hl], Cst[d]], [pn])
            pu = psU[d]
            P.mm(pu[:, 0:130], kt_[:, :], v_[:, :], True, True, [kt_, v_], [pu])
            P.op("dve", lambda e, d=d, pu=pu: e.tensor_tensor(Cst[d][:, :], pu[:, 0:130], Cst[d][:, :], ALU.add), [pu, Cst[d]], [Cst[d]])
            if 0 <= cn < NCH:
                P.op("dve", lambda e, d=d, cn=cn: e.tensor_scalar(Cst[d][:, :], Cst[d][:, :], egcol[d][:, cn:cn + 1], None, ALU.mult),
                     [Cst[d], egcol[d]], [Cst[d]])
            a0, a1, a2 = sm[d]
            pnv = pn[:, 0:256].rearrange("p (h x) -> p h x", h=2)
            P.op("dve", lambda e, pnv=pnv, c=c, d=d, a0=a0: e.tensor_tensor(a0[:, :], pnv[:, :, 64], eq[:, c, d * 2:d * 2 + 2], ALU.mult),
                 [pn, eq], [a0])
            P.op("dve", lambda e, a0=a0, a1=a1: e.tensor_scalar(a1[:, :], a0[:, :], -1.0, 1.0, ALU.mult, ALU.max), [a0], [a1])
            P.op("dve", lambda e, a0=a0, a1=a1: e.scalar_tensor_tensor(a1[:, :], a0[:, :], 1.0, a1[:, :], ALU.max, ALU.max), [a0, a1], [a1])
            P.op("dve", lambda e, a1=a1, a2=a2: e.reciprocal(a2[:, :], a1[:, :]), [a1], [a2])
            P.op("dve", lambda e, a2=a2, c=c, d=d: e.tensor_tensor(a2[:, :], a2[:, :], eq[:, c, d * 2:d * 2 + 2], ALU.mult), [a2, eq], [a2])
            for hl in range(2):
                P.op("dve", lambda e, hl=hl, c=c, pn=pn, a2=a2: e.scalar_tensor_tensor(
                    Hs[:, c, hl * 64:(hl + 1) * 64], pn[:, hl * 128:hl * 128 + 64], a2[:, hl:hl + 1],
                    Hs[:, c, hl * 64:(hl + 1) * 64], ALU.mult, ALU.add), [pn, a2, Hs], [Hs])
    sq = bufA
    P.op("dve", lambda e: e.tensor_tensor(sq[:, 0:S], Hs[:, :, :].rearrange("p c x -> p (c x)"), Hs[:, :, :].rearrange("p c x -> p (c x)"), ALU.mult),
         [Hs], [sq])
    ss = P.sb([128, NCH * 2], F32, "ss")
    P.op("dve", lambda e: e.tensor_reduce(ss[:, :], sq[:, 0:S].rearrange("p (a x) -> p a x", x=64), AX.X, ALU.add), [sq], [ss])
    P.op("act", lambda e: e.activation(ss[:, :], ss[:, :], AF.Sqrt, bias=eps_b[:, 0:1], scale=1.0 / 64), [ss, eps_b], [ss])
    P.op("dve", lambda e: e.reciprocal(ss[:, :], ss[:, :]), [ss], [ss])
    for c in range(NCH):
        for hl in range(2):
            P.op("dve" if (c + hl) % 2 else "pool", lambda e, c=c, hl=hl: e.tensor_scalar(
                Hs[:, c, hl * 64:(hl + 1) * 64], Hs[:, c, hl * 64:(hl + 1) * 64], ss[:, c * 2 + hl:c * 2 + hl + 1], None, ALU.mult),
                [Hs, ss], [Hs])
    hnT = qTm[0]
    for g4 in range(NCH // 4):
        pt = pp[g4 % 2]
        for j in range(4):
            c = g4 * 4 + j
            P.op("pe", lambda e, pt=pt, j=j, c=c: e.transpose(pt[:, j * 128:(j + 1) * 128], Hs[:, c, :], idn[:, :]), [Hs, idn], [pt])
        P.op("dve", lambda e, pt=pt, g4=g4: e.tensor_scalar(hnT[:, g4 * 512:(g4 + 1) * 512], pt[:, :], cw_sb[:, 6:7], None, ALU.mult),
             [pt, cw_sb], [hnT])
    mo = bufA
    P.dma_("sp", mo[:, 0:S], moT, [], [mo])
    P.op("act", lambda e: e.activation(mo[:, 0:S], mo[:, 0:S], AF.Sigmoid), [mo], [mo])
    P.op("dve", lambda e: e.scalar_tensor_tensor(hnT[:, :], xc[:, :], cw_sb[:, 7:8], hnT[:, :], ALU.mult, ALU.add), [xc, cw_sb, hnT], [hnT])
    P.op("dve", lambda e: e.tensor_tensor(hnT[:, :], hnT[:, :], mo[:, 0:S], ALU.mult), [hnT, mo], [hnT])
    P.dma_("sp", yT, hnT[:, :], [hnT], [])
    P.emit()
    return nc


_PERM = np.concatenate([np.arange(32, 64), np.arange(0, 32)])
_PROGS = {}


def _prog(key, builder):
    if key not in _PROGS:
        nc = bass.Bass("TRN2", target_bir_lowering=False)
        builder(nc)
        _PROGS[key] = nc
    return _PROGS[key]


def _arr(g, kc):
    return np.ascontiguousarray(np.asarray(g, np.float32).reshape(kc, 128).T)


def _c(a):
    return np.ascontiguousarray(a, dtype=np.float32)


def kernel(x, p, positions, ffn1_norm, ffn1_w_gate, ffn1_w_up, ffn1_w_down, mix_norm, w_in,
           mla_q_norm, mla_w_uq, mla_kv_norm, mla_w_ukv, mlstm_conv_w, mlstm_conv_b, mlstm_w_q, mlstm_w_k,
           mlstm_i_bias, mlstm_f_bias, mlstm_head_norm, mlstm_skip, fnet_w, fnet_b, w_out,
           ffn2_norm, ffn2_w_gate, ffn2_w_up, ffn2_w_down, ple_gate_norm, ple_w_gate, ple_w_proj,
           ple_post_norm, final_norm):
    x = np.asarray(x, np.float32)
    p = np.asarray(p, np.float32)
    positions = np.asarray(positions, np.int32)
    NCORE, HALF, S = 8, 4096, 8192
    cores = list(range(NCORE))
    hT = [_c(x[c // 2, (c % 2) * HALF:(c % 2 + 1) * HALF].T) for c in cores]
    inv = (1.0 / (10000.0 ** (np.arange(0, 64, 2, dtype=np.float32) / 64))).astype(np.float32)
    rc = np.stack([np.concatenate([inv, inv]), np.concatenate([-np.ones(32), np.ones(32)])], 1).astype(np.float32)
    depth = w_in.shape[0]
    for i in range(depth):
        wi = np.asarray(w_in[i], np.float32)
        win = _c(np.concatenate([wi[:, 0:384], wi[:, 384:640], wi[:, 640:704], wi[:, 640:704][:, _PERM],
                                 wi[:, 704:960], wi[:, 960:1216], wi[:, 1216:1472], wi[:, 1488:1744],
                                 wi[:, 1472:1488]], 1))
        ncA = _prog("A", lambda nc: build_phaseA(nc, HALF))
        common = dict(g1=_arr(ffn1_norm[i], 8), gm=_arr(mix_norm[i], 8), wg=_c(ffn1_w_gate[i]), wu=_c(ffn1_w_up[i]),
                      wd=_c(ffn1_w_down[i]), win=win)
        res = run_bass_kernel_spmd(ncA, [dict(hT=hT[c], **common) for c in cores], core_ids=cores).results
        hT = [res[c]["hT_o"] for c in cores]
        uT = [np.concatenate([res[2 * b]["uT_o"], res[2 * b + 1]["uT_o"]], 1) for b in range(4)]
        ncM = _prog("MLA", lambda nc: build_mla(nc, S))
        wq4 = np.asarray(mla_w_uq[i], np.float32).reshape(384, 4, 192)
        wkv4 = np.asarray(mla_w_ukv[i], np.float32).reshape(256, 4, 256)
        maps = []
        for c in cores:
            b, heads = c // 2, (2 * (c % 2), 2 * (c % 2) + 1)
            wq = np.concatenate([np.concatenate([wq4[:, h, :128], wq4[:, h, 128:], wq4[:, h, 128:][:, _PERM]], 1)
                                 for h in heads], 1)
            wkv = np.concatenate([wkv4[:, h, :128] for h in heads] + [wkv4[:, h, 128:] for h in heads], 1)
            maps.append(dict(cqT=_c(uT[b][0:384]), ckvT=_c(uT[b][384:640]), krT=_c(uT[b][640:768]),
                             pos=np.ascontiguousarray(positions[b][None, :]), wq=_c(wq), wkv=_c(wkv),
                             gq=_arr(mla_q_norm[i], 3), gkv=_arr(mla_kv_norm[i], 2), rc=rc))
        resM = run_bass_kernel_spmd(ncM, maps, core_ids=cores).results
        yT = []
        for b in range(4):
            y = np.zeros((1024, S), np.float32)
            y[0:256] = resM[2 * b]["yT"]
            y[256:512] = resM[2 * b + 1]["yT"]
            yT.append(y)
        ncL = _prog("MLSTM", lambda nc: build_mlstm(nc, S // 128))
        lc = mlstm_consts()
        cwq, cwk = np.asarray(mlstm_w_q[i], np.float32), np.asarray(mlstm_w_k[i], np.float32)
        ib, fbias = np.asarray(mlstm_i_bias[i], np.float32), np.asarray(mlstm_f_bias[i], np.float32)
        cvw, cvb = np.asarray(mlstm_conv_w[i], np.float32), np.asarray(mlstm_conv_b[i], np.float32)
        hnorm, skp = np.asarray(mlstm_head_norm[i], np.float32), np.asarray(mlstm_skip[i], np.float32)
        maps = []
        for c in cores:
            b, hp = c // 2, c % 2
            cs = slice(hp * 128, (hp + 1) * 128)
            heads = [2 * hp, 2 * hp + 1]
            mif = uT[b][1792:1808].T.reshape(S, 2, 2, 4)
            G = mif[:, :, :, heads].reshape(S // 128, 128, 8).transpose(1, 0, 2)
            gbv = np.stack([ib[:, heads], fbias[:, heads]], 0).reshape(1, 8)
            cwm = np.concatenate([cvw[:, cs].T, cvb[cs][:, None], hnorm[cs][:, None], skp[cs][:, None]], 1)
            wqm = np.zeros((128, 256), np.float32)
            wqm[:64, 0:64] = cwq[heads[0]]
            wqm[64:, 192:256] = cwq[heads[1]]
            wkb = np.zeros((128, 128), np.float32)
            wkb[:64, :64] = cwk[heads[0]]
            wkb[64:, 64:] = cwk[heads[1]]
            maps.append(dict(mxT=_c(uT[b][768 + hp * 128:768 + (hp + 1) * 128]),
                             moT=_c(uT[b][1280 + hp * 128:1280 + (hp + 1) * 128]),
                             mv=_c(uT[b][1024 + hp * 128:1024 + (hp + 1) * 128].T.reshape(S // 128, 128, 128).transpose(1, 0, 2)),
                             G=_c(G), gb=_c(np.repeat(gbv, 128, 0)), cw=_c(cwm), WqBD=wqm, WkBD=wkb, **lc))
        resL = run_bass_kernel_spmd(ncL, maps, core_ids=cores).results
        ncF = _prog("FNET", build_fnet)
        fc = fnet_consts()
        fw, fbv = np.asarray(fnet_w[i], np.float32), np.asarray(fnet_b[i], np.float32)
        maps = []
        for c in cores:
            b, hp = c // 2, c % 2
            wbd = np.zeros((128, 128), np.float32)
            wbd[:64, :64] = fw[2 * hp]
            wbd[64:, 64:] = fw[2 * hp + 1]
            z = uT[b][1536 + hp * 128:1536 + (hp + 1) * 128].T
            maps.append(dict(Z1=_c(z.reshape(128, 64, 128)), WBD=wbd, fb=_c(fbv[hp * 128:(hp + 1) * 128][:, None]), **fc))
        resF = run_bass_kernel_spmd(ncF, maps, core_ids=cores).results
        for c in cores:
            b, hp = c // 2, c % 2
            yT[b][512 + hp * 128:512 + (hp + 1) * 128] = resL[c]["yT"]
            yT[b][768 + hp * 128:768 + (hp + 1) * 128] = resF[c]["yT"]
        last = (i == depth - 1)
        ncC = _prog("C%d" % int(last), lambda nc: build_phaseC(nc, HALF, last))
        common = dict(wout=_c(w_out[i]), g2=_arr(ffn2_norm[i], 8), wg=_c(ffn2_w_gate[i]), wu=_c(ffn2_w_up[i]),
                      wd=_c(ffn2_w_down[i]), gg=_arr(ple_gate_norm[i], 8), wgate=_c(ple_w_gate[i]),
                      wproj=_c(ple_w_proj[i]), gp=_arr(ple_post_norm[i], 8), gf=_arr(final_norm, 8))
        maps = []
        for c in cores:
            b, sl = c // 2, slice((c % 2) * HALF, (c % 2 + 1) * HALF)
            maps.append(dict(hT=hT[c], yT=_c(yT[b][:, sl]), pT=_c(p[i, b, sl].T), **common))
        res = run_bass_kernel_spmd(ncC, maps, core_ids=cores).results
        hT = [res[c]["hT_o"] for c in cores]
    out = np.empty((4, S, 1024), np.float32)
    for c in cores:
        out[c // 2, (c % 2) * HALF:(c % 2 + 1) * HALF] = hT[c].T
    return out
```
